# Optimizing a Trainium2 kernel written in Bass

```python
import math
import jax, jax.numpy as jnp
from jax import lax
import numpy as np

D_MODEL = 1024
BATCH = 4
SEQ = 4096
DEPTH = 4

N_EVEN = (DEPTH + 1) // 2
N_ODD = DEPTH // 2
CHUNK = 128
EPS = 1e-6
F32 = jnp.float32

RET_HEADS = 4
RET_DK = 256
RET_DV = 256
RET_QK = RET_HEADS * RET_DK
RET_V = RET_HEADS * RET_DV
ROPE_BASE = 10000.0

SSD_INNER = D_MODEL
SSD_HEADDIM = 64
SSD_HEADS = SSD_INNER // SSD_HEADDIM
SSD_GROUPS = 2
SSD_HPG = SSD_HEADS // SSD_GROUPS
SSD_STATE = 128
SSD_CONV = 4
SSD_CONV_CH = SSD_INNER + 2 * SSD_GROUPS * SSD_STATE

IN_SIZES = (RET_QK, RET_QK, RET_V, RET_V, SSD_INNER, SSD_CONV_CH, SSD_HEADS)
IN_PROJ = sum(IN_SIZES)
MIX_WIDTH = RET_V + SSD_INNER

S5_GROUP = 16
S5_GROUPS = D_MODEL // S5_GROUP
S5_STATE = 64

D_FF = 4 * D_MODEL

kernel_name = "hybrid_retention_ssd_s5_trunk"


def rms_norm(x, w):
    x32 = x.astype(F32)
    y = x32 * lax.rsqrt(jnp.mean(x32 * x32, axis=-1, keepdims=True) + EPS)
    return (y * w.astype(F32)).astype(x.dtype)


def rotary(x, pos):
    d = x.shape[-1]
    half = d // 2
    inv = ROPE_BASE ** (-jnp.arange(half, dtype=F32) / half)
    ang = pos.astype(F32)[:, None] * inv[None, :]
    cos = jnp.cos(ang)[None, :, None, :]
    sin = jnp.sin(ang)[None, :, None, :]
    x1, x2 = x[..., :half], x[..., half:]
    return jnp.concatenate([x1 * cos - x2 * sin, x2 * cos + x1 * sin], axis=-1)


def retention(q, k, v):
    b, L, h, dk = q.shape
    dv = v.shape[-1]
    nc = L // CHUNK
    pos = jnp.arange(L)
    q = rotary(q, pos)
    k = rotary(k, pos) * (dk ** -0.5)
    log_g = jnp.log1p(-(2.0 ** (-5.0 - jnp.arange(h, dtype=F32))))
    idx = jnp.arange(CHUNK, dtype=F32)
    rel = idx[:, None] - idx[None, :]
    decay = jnp.where(rel[None] >= 0, jnp.exp(jnp.maximum(rel, 0.0)[None] * log_g[:, None, None]), 0.0)
    qc = q.reshape(b, nc, CHUNK, h, dk)
    kc = k.reshape(b, nc, CHUNK, h, dk)
    vc = v.reshape(b, nc, CHUNK, h, dv)
    scores = jnp.einsum('bcnhd,bcmhd->bchnm', qc, kc) * decay[None, None]
    y_in = jnp.einsum('bchnm,bcmhe->bcnhe', scores, vc)
    zeta = jnp.exp((CHUNK - 1.0 - idx)[None, :] * log_g[:, None])
    xi = jnp.exp((idx + 1.0)[None, :] * log_g[:, None]).T
    g_chunk = jnp.exp(CHUNK * log_g)

    def step(R, inp):
        qi, ki, vi = inp
        y = jnp.einsum('bnhd,bhde->bnhe', qi, R) * xi[None, :, :, None]
        R = g_chunk[None, :, None, None] * R + jnp.einsum('bmhd,hm,bmhe->bhde', ki, zeta, vi)
        return R, y

    R0 = jnp.zeros((b, h, dk, dv), F32)
    _, y_x = lax.scan(step, R0, (qc.swapaxes(0, 1), kc.swapaxes(0, 1), vc.swapaxes(0, 1)))
    y = (y_in + y_x.swapaxes(0, 1)).reshape(b, L, h, dv)
    mu = jnp.mean(y, axis=-1, keepdims=True)
    var = jnp.mean(jnp.square(y - mu), axis=-1, keepdims=True)
    return (y - mu) * lax.rsqrt(var + EPS)


def causal_dwconv(x, w, bias):
    K, ch = w.shape
    y = lax.conv_general_dilated(x, w[:, None, :], window_strides=(1,), padding=[(K - 1, 0)],
                                 dimension_numbers=('NWC', 'WIO', 'NWC'), feature_group_count=ch)
    return y + bias


def ssd(xs, Bm, Cm, dt, a_log, d_skip):
    b, L, G, R, P = xs.shape
    N = Bm.shape[-1]
    nc = L // CHUNK
    A = -jnp.exp(a_log)
    dA = (dt * A).reshape(b, nc, CHUNK, G, R).transpose(0, 3, 4, 1, 2)
    X = (xs * dt.reshape(b, L, G, R)[..., None]).reshape(b, nc, CHUNK, G, R, P)
    Bc = Bm.reshape(b, nc, CHUNK, G, N)
    Cc = Cm.reshape(b, nc, CHUNK, G, N)
    cs = jnp.cumsum(dA, axis=-1)
    tril = jnp.tril(jnp.ones((CHUNK, CHUNK), dtype=bool))
    seg = cs[..., :, None] - cs[..., None, :]
    Lmat = jnp.where(tril, jnp.exp(jnp.minimum(seg, 0.0)), 0.0)
    CB = jnp.einsum('bclgn,bcsgn->bgcls', Cc, Bc)
    M = CB[:, :, None] * Lmat
    y_diag = jnp.einsum('bgrcls,bcsgrp->bclgrp', M, X)
    decay_st = jnp.exp(cs[..., -1:] - cs)
    states = jnp.einsum('bclgn,bgrcl,bclgrp->cbgrpn', Bc, decay_st, X)
    chunk_decay = jnp.exp(cs[..., -1]).transpose(3, 0, 1, 2)

    def step(S, inp):
        st, dec = inp
        return dec[..., None, None] * S + st, S

    S0 = jnp.zeros((b, G, R, P, N), F32)
    _, prev = lax.scan(step, S0, (states, chunk_decay))
    y_off = jnp.einsum('bclgn,cbgrpn,bgrcl->bclgrp', Cc, prev, jnp.exp(cs))
    y = (y_diag + y_off).reshape(b, L, G, R, P) + d_skip.reshape(G, R)[..., None] * xs
    return y.reshape(b, L, G * R * P)


def ret_ssd_mixer(h, w_in, conv_w, conv_b, dt_bias, a_log, d_ssd, ssd_norm, w_out):
    b, L, _ = h.shape
    proj = (h @ w_in).astype(F32)
    splits = [int(s) for s in np.cumsum(IN_SIZES)[:-1]]
    q, k, v, g, z, xbc, dt = jnp.split(proj, splits, axis=-1)
    yr = retention(q.reshape(b, L, RET_HEADS, RET_DK), k.reshape(b, L, RET_HEADS, RET_DK),
                   v.reshape(b, L, RET_HEADS, RET_DV))
    yr = jax.nn.silu(g) * yr.reshape(b, L, RET_V)
    xbc = jax.nn.silu(causal_dwconv(xbc, conv_w.astype(F32), conv_b.astype(F32)))
    xs, Bm, Cm = jnp.split(xbc, [SSD_INNER, SSD_INNER + SSD_GROUPS * SSD_STATE], axis=-1)
    dt = jax.nn.softplus(dt + dt_bias.astype(F32))
    ys = ssd(xs.reshape(b, L, SSD_GROUPS, SSD_HPG, SSD_HEADDIM),
             Bm.reshape(b, L, SSD_GROUPS, SSD_STATE), Cm.reshape(b, L, SSD_GROUPS, SSD_STATE),
             dt, a_log.astype(F32), d_ssd.astype(F32))
    ys = (ys * jax.nn.silu(z)).reshape(b, L, SSD_GROUPS, SSD_INNER // SSD_GROUPS)
    ys = ys * lax.rsqrt(jnp.mean(ys * ys, axis=-1, keepdims=True) + EPS)
    ys = ys.reshape(b, L, SSD_INNER) * ssd_norm.astype(F32)
    y = jnp.concatenate([yr, ys], axis=-1).astype(h.dtype)
    return y @ w_out


def s5_mixer(h, lam_re, lam_im, log_step, b_re, b_im, c_re, c_im, d_s5, w_glu_a, w_glu_b):
    b, L, _ = h.shape
    h32 = h.astype(F32)
    u = h32.reshape(b, L, S5_GROUPS, S5_GROUP)
    lr = jnp.minimum(lam_re.astype(F32), -1e-4)
    li = lam_im.astype(F32)
    delta = jnp.exp(log_step.astype(F32))[:, None]
    mag = jnp.exp(lr * delta)
    ab_re = mag * jnp.cos(li * delta)
    ab_im = mag * jnp.sin(li * delta)
    den = lr * lr + li * li
    nr, ni = ab_re - 1.0, ab_im
    coef_re = ((nr * lr + ni * li) / den)[..., None]
    coef_im = ((ni * lr - nr * li) / den)[..., None]
    br, bi = b_re.astype(F32), b_im.astype(F32)
    bb_re = coef_re * br - coef_im * bi
    bb_im = coef_re * bi + coef_im * br
    bu_re = jnp.einsum('blgc,gpc->lbgp', u, bb_re)
    bu_im = jnp.einsum('blgc,gpc->lbgp', u, bb_im)
    a_re = jnp.broadcast_to(ab_re[None, None], (L, 1, S5_GROUPS, S5_STATE))
    a_im = jnp.broadcast_to(ab_im[None, None], (L, 1, S5_GROUPS, S5_STATE))

    def combine(e1, e2):
        a1r, a1i, b1r, b1i = e1
        a2r, a2i, b2r, b2i = e2
        return (a2r * a1r - a2i * a1i, a2r * a1i + a2i * a1r,
                a2r * b1r - a2i * b1i + b2r, a2r * b1i + a2i * b1r + b2i)

    _, _, xr, xi = lax.associative_scan(combine, (a_re, a_im, bu_re, bu_im), axis=0)
    y = (jnp.einsum('lbgp,gcp->blgc', xr, c_re.astype(F32))
         - jnp.einsum('lbgp,gcp->blgc', xi, c_im.astype(F32)))
    y = y.reshape(b, L, D_MODEL) + d_s5.astype(F32) * h32
    y = jax.nn.gelu(y).astype(h.dtype)
    return (y @ w_glu_a) * jax.nn.sigmoid(y @ w_glu_b)


def setup_inputs(seed: int = 0) -> dict:
    key = jax.random.key(seed)
    ks = jax.random.split(key, 32)
    nrm = lambda k, s, sc: jax.random.normal(k, s, F32) * sc
    gain = lambda k, s: 1.0 + 0.02 * jax.random.normal(k, s, F32)
    x = jax.random.normal(ks[0], (BATCH, SEQ, D_MODEL), F32)
    dt0 = jnp.exp(jax.random.uniform(ks[8], (N_EVEN, SSD_HEADS), F32, math.log(1e-3), math.log(1e-1)))
    lam_im0 = math.pi * jnp.arange(S5_STATE, dtype=F32)
    return {
        "x": x,
        "pre_mix_norm": gain(ks[1], (DEPTH, D_MODEL)),
        "post_mix_norm": gain(ks[2], (DEPTH, D_MODEL)),
        "pre_mlp_norm": gain(ks[3], (DEPTH, D_MODEL)),
        "post_mlp_norm": gain(ks[4], (DEPTH, D_MODEL)),
        "w_in": nrm(ks[5], (N_EVEN, D_MODEL, IN_PROJ), D_MODEL ** -0.5),
        "conv_w": nrm(ks[6], (N_EVEN, SSD_CONV, SSD_CONV_CH), SSD_CONV ** -0.5),
        "conv_b": nrm(ks[7], (N_EVEN, SSD_CONV_CH), 0.02),
        "dt_bias": dt0 + jnp.log(-jnp.expm1(-dt0)),
        "a_log": jnp.log(jax.random.uniform(ks[9], (N_EVEN, SSD_HEADS), F32, 1.0, 16.0)),
        "d_ssd": 1.0 + 0.1 * jax.random.normal(ks[10], (N_EVEN, SSD_HEADS), F32),
        "ssd_norm": gain(ks[11], (N_EVEN, SSD_INNER)),
        "w_out": nrm(ks[12], (N_EVEN, MIX_WIDTH, D_MODEL), MIX_WIDTH ** -0.5),
        "lam_re": -0.5 + 0.01 * jax.random.normal(ks[13], (N_ODD, S5_GROUPS, S5_STATE), F32),
        "lam_im": lam_im0 + 0.01 * jax.random.normal(ks[14], (N_ODD, S5_GROUPS, S5_STATE), F32),
        "log_step": jax.random.uniform(ks[15], (N_ODD, S5_GROUPS), F32, math.log(1e-3), math.log(1e-1)),
        "b_re": nrm(ks[16], (N_ODD, S5_GROUPS, S5_STATE, S5_GROUP), (2 * S5_GROUP) ** -0.5),
        "b_im": nrm(ks[17], (N_ODD, S5_GROUPS, S5_STATE, S5_GROUP), (2 * S5_GROUP) ** -0.5),
        "c_re": nrm(ks[18], (N_ODD, S5_GROUPS, S5_GROUP, S5_STATE), S5_STATE ** -0.5),
        "c_im": nrm(ks[19], (N_ODD, S5_GROUPS, S5_GROUP, S5_STATE), S5_STATE ** -0.5),
        "d_s5": nrm(ks[20], (N_ODD, D_MODEL), 1.0),
        "w_glu_a": nrm(ks[21], (N_ODD, D_MODEL, D_MODEL), D_MODEL ** -0.5),
        "w_glu_b": nrm(ks[22], (N_ODD, D_MODEL, D_MODEL), D_MODEL ** -0.5),
        "w_up": nrm(ks[23], (DEPTH, D_MODEL, D_FF), D_MODEL ** -0.5),
        "w_down": nrm(ks[24], (DEPTH, D_FF, D_MODEL), D_FF ** -0.5),
    }


def reference(x, pre_mix_norm, post_mix_norm, pre_mlp_norm, post_mlp_norm, w_in, conv_w, conv_b,
              dt_bias, a_log, d_ssd, ssd_norm, w_out, lam_re, lam_im, log_step, b_re, b_im,
              c_re, c_im, d_s5, w_glu_a, w_glu_b, w_up, w_down):
    h = x
    for i in range(DEPTH):
        j = i // 2
        hn = rms_norm(h, pre_mix_norm[i])
        if i % 2 == 0:
            m = ret_ssd_mixer(hn, w_in[j], conv_w[j], conv_b[j], dt_bias[j], a_log[j], d_ssd[j],
                              ssd_norm[j], w_out[j])
        else:
            m = s5_mixer(hn, lam_re[j], lam_im[j], log_step[j], b_re[j], b_im[j], c_re[j], c_im[j],
                         d_s5[j], w_glu_a[j], w_glu_b[j])
        h = h + rms_norm(m, post_mix_norm[i])
        hn = rms_norm(h, pre_mlp_norm[i])
        f = jnp.square(jax.nn.relu(hn @ w_up[i])) @ w_down[i]
        h = h + rms_norm(f, post_mlp_norm[i])
    return h
```

```python
import numpy as np
import concourse.bass as bass
import concourse.mybir as mybir
from concourse.bass_utils import run_bass_kernel_spmd

F32 = mybir.dt.float32
BF16 = mybir.dt.bfloat16
ALU = mybir.AluOpType
AF = mybir.ActivationFunctionType
AX = mybir.AxisListType

_DSZ = {F32: 4, BF16: 2}


def _dsize(dt):
    if dt in _DSZ:
        return _DSZ[dt]
    s = str(dt)
    if "64" in s:
        return 8
    if "32" in s:
        return 4
    if "16" in s:
        return 2
    return 1


class _Op:
    __slots__ = ("eng", "fn", "waits", "signal", "idx", "dma_sem", "dma_cnt", "cnt")

    def __init__(self, eng, fn):
        self.eng = eng
        self.fn = fn
        self.waits = []
        self.signal = False
        self.idx = -1
        self.dma_sem = None
        self.dma_cnt = 0
        self.cnt = 0


class Sched:
    ENGS = ("pe", "act", "dve", "pool", "sp")
    NDMA = 24
    EPOCH = 30000

    def __init__(self, nc):
        self.nc = nc
        self.ops = {e: [] for e in self.ENGS}
        self.segs = {}
        self.seen = {e: {} for e in self.ENGS}
        self.dma_n = 0
        self.dma_last = [None] * self.NDMA
        self.dma_count = [0] * self.NDMA
        self.nsig = {e: 0 for e in self.ENGS}
        self.untracked = set()

    def _region(self, ap):
        t = ap.tensor
        name = t.name
        if name in self.untracked:
            return None
        dims = ap.ap
        esz = _dsize(ap.dtype)
        off = int(ap.offset)
        sp = str(ap.space) if hasattr(ap, "space") else ""
        if "DRAM" in sp.upper() or "Dram" in sp or "dram" in sp:
            lo = off
            hi = off
            for st, cn in dims:
                hi += abs(st) * (cn - 1)
            return (name, 0, 1, lo * esz, (hi + 1) * esz)
        if "PSUM" in sp.upper():
            return (name, 0, 128, 0, 1 << 30)
        pstep = dims[0][0]
        pcount = dims[0][1]
        if pstep == 0:
            pstep = 1 << 40
        p0 = off // pstep
        f0 = off - p0 * pstep
        ext = 0
        for st, cn in dims[1:]:
            ext += abs(st) * (cn - 1)
        return (name, p0, p0 + pcount, f0 * esz, (f0 + ext + 1) * esz)

    @staticmethod
    def _addr(d, op):
        key = op.eng if op.dma_sem is None else ("dma", op.dma_sem)
        d[key] = op

    def _deps(self, op, reads, writes):
        deps = []
        psum_reads = [ap for ap in reads if "PSUM" in str(ap.space).upper()]
        if psum_reads:
            reads = [ap for ap in reads if "PSUM" not in str(ap.space).upper()]
            writes = list(writes) + psum_reads
        for ap in reads:
            rg = self._region(ap)
            if rg is None:
                continue
            name, p0, p1, b0, b1 = rg
            for s in self.segs.get(name, ()):
                if s[0] < p1 and p0 < s[1] and s[2] < b1 and b0 < s[3]:
                    if s[4] is not None:
                        deps.append(s[4])
                    self._addr(s[5], op)
        for ap in writes:
            rg = self._region(ap)
            if rg is None:
                continue
            name, p0, p1, b0, b1 = rg
            lst = self.segs.setdefault(name, [])
            keep = []
            for s in lst:
                if s[0] < p1 and p0 < s[1] and s[2] < b1 and b0 < s[3]:
                    if s[4] is not None:
                        deps.append(s[4])
                    deps.extend(s[5].values())
                    if p0 <= s[0] and s[1] <= p1 and b0 <= s[2] and s[3] <= b1:
                        continue
                    s[5] = {}
                    keep.append(s)
                else:
                    keep.append(s)
            keep.append([p0, p1, b0, b1, op, {}])
            self.segs[name] = keep
        return deps

    def _add_waits(self, op, prods):
        e = op.eng
        best = {}
        for prod in prods:
            if prod is op:
                continue
            if prod.dma_sem is not None:
                key = ("dma", prod.dma_sem)
                val = prod.dma_cnt
            else:
                if prod.eng == "pe" and e == "pe":
                    continue
                key = prod.eng
                val = prod.idx
            b = best.get(key)
            if b is None or b[0] < val:
                best[key] = (val, prod)
        seen = self.seen[e]
        for key, (val, prod) in best.items():
            if seen.get(key, -1) >= val:
                continue
            seen[key] = val
            if prod.dma_sem is None:
                prod.signal = True
            op.waits.append((key, prod))

    def _rec(self, eng, fn, r, w):
        op = _Op(eng, fn)
        op.idx = len(self.ops[eng])
        self._add_waits(op, self._deps(op, r, w))
        self.ops[eng].append(op)
        return op

    def pe(self, fn, r=(), w=()):
        return self._rec("pe", fn, r, w)

    def act(self, fn, r=(), w=()):
        return self._rec("act", fn, r, w)

    def dve(self, fn, r=(), w=()):
        return self._rec("dve", fn, r, w)

    def pool(self, fn, r=(), w=()):
        return self._rec("pool", fn, r, w)

    def dma(self, out, in_, q="sp", **kw):
        j = self.dma_n % self.NDMA
        self.dma_n += 1
        fn = (lambda e, o=out, i=in_, k=kw: e.dma_start(out=o, in_=i, **k))
        op = _Op(q, fn)
        op.idx = len(self.ops[q])
        prev = self.dma_last[j]
        self.dma_count[j] += 1
        op.dma_sem = j
        op.dma_cnt = self.dma_count[j]
        deps = self._deps(op, [in_], [out])
        if prev is not None:
            deps.append(prev)
        self._add_waits(op, deps)
        self.dma_last[j] = op
        self.ops[q].append(op)
        return op

    def emit(self, final_waits=()):
        nc = self.nc
        for e in self.ENGS:
            c = 0
            for op in self.ops[e]:
                if op.signal and op.dma_sem is None:
                    c += 1
                op.cnt = c
            self.nsig[e] = c
        nep = {e: max(1, (self.nsig[e] + self.EPOCH - 1) // self.EPOCH) for e in self.ENGS}
        import contextlib
        with contextlib.ExitStack() as st:
            esem = {e: [st.enter_context(nc.semaphore("s_%s%d" % (e, k))) for k in range(nep[e])]
                    for e in self.ENGS}
            dsem = [st.enter_context(nc.semaphore("s_dma%d" % k)) for k in range(self.NDMA)]
            block = st.enter_context(nc.Block())
            EP = self.EPOCH

            def run(ename, eng):
                for op in self.ops[ename]:
                    for key, prod in op.waits:
                        if isinstance(key, tuple):
                            eng.wait_ge(dsem[key[1]], 16 * prod.dma_cnt)
                        else:
                            c = prod.cnt
                            ep = (c - 1) // EP
                            eng.wait_ge(esem[key][ep], c - ep * EP)
                    ins = op.fn(eng)
                    if op.dma_sem is not None:
                        ins.then_inc(dsem[op.dma_sem], 16)
                    elif op.signal:
                        ep = (op.cnt - 1) // EP
                        ins.then_inc(esem[ename][ep], 1)
                if ename == "sp":
                    for j in range(self.NDMA):
                        if self.dma_count[j] > 0:
                            eng.wait_ge(dsem[j], 16 * self.dma_count[j])

            @block.tensor
            def _(eng):
                run("pe", eng)

            @block.scalar
            def _(eng):
                run("act", eng)

            @block.vector
            def _(eng):
                run("dve", eng)

            @block.gpsimd
            def _(eng):
                run("pool", eng)

            @block.sync
            def _(eng):
                run("sp", eng)


D = 1024
KT = 8
DFF = 4096
FT = 32
BLK = 256
CH = 128
EPS = 1e-6
NCORES = 8
INP = 6672
MIXW = 2048


class Prog:
    def __init__(self, L, layers=(0, 1, 2, 3), mixers=True):
        self.L = L
        self.layers = list(layers)
        self.mixers = mixers
        self.NB = L // BLK
        self.nlw = len(self.layers)
        self.wi = {li: i for i, li in enumerate(self.layers)}
        self.nc = bass.Bass("TRN2", target_bir_lowering=False)
        self.S = Sched(self.nc)
        self.din = {}

    def dbg(self, name, ap, dt=F32):
        if not getattr(self, "debug", False):
            return
        n = ap.shape[1]
        o = self.nc.dram_tensor("dbg_" + name, [ap.shape[0], n], dt, kind="ExternalOutput").ap()
        self.S.dma(o, ap)

    def inp(self, name, shape, dt=F32):
        ap = self.nc.dram_tensor(name, list(shape), dt, kind="ExternalInput").ap()
        self.S.untracked.add(name)
        self.din[name] = ap
        return ap

    def scratch(self, name, shape, dt):
        return self.nc.dram_tensor(name, list(shape), dt, kind="Internal").ap()

    def mm(self, out, lhsT, rhs, start=True, stop=True, **kw):
        self.S.pe(lambda e: e.matmul(out, lhsT=lhsT, rhs=rhs, start=start, stop=stop, **kw), r=[lhsT, rhs], w=[out])

    def tr(self, out, in_, ident):
        self.S.pe(lambda e: e.transpose(out=out, in_=in_, identity=ident), r=[in_, ident], w=[out])

    def actf(self, out, in_, func, r=(), **kw):
        rr = [in_] + list(r)
        for k in ("scale", "bias"):
            if k in kw and not isinstance(kw[k], (int, float)):
                rr.append(kw[k])
        ww = [out]
        if kw.get("accum_out") is not None:
            ww.append(kw["accum_out"])
        self.S.act(lambda e: e.activation(out=out, in_=in_, func=func, **kw), r=rr, w=ww)

    def tt(self, out, in0, in1, op, eng="dve"):
        f = lambda e: e.tensor_tensor(out=out, in0=in0, in1=in1, op=op)
        (self.S.dve if eng == "dve" else self.S.pool)(f, r=[in0, in1], w=[out])

    def ts(self, out, in0, s1, op0, s2=None, op1=None, eng="dve"):
        rr = [in0] + [s for s in (s1, s2) if s is not None and not isinstance(s, (int, float))]
        if op1 is None:
            f = lambda e: e.tensor_scalar(out=out, in0=in0, scalar1=s1, scalar2=None, op0=op0)
        else:
            f = lambda e: e.tensor_scalar(out=out, in0=in0, scalar1=s1, scalar2=s2, op0=op0, op1=op1)
        (self.S.dve if eng == "dve" else self.S.pool)(f, r=rr, w=[out])

    def stt(self, out, in0, scalar, in1, op0, op1, eng="dve"):
        rr = [in0, in1] + ([scalar] if not isinstance(scalar, (int, float)) else [])
        f = lambda e: e.scalar_tensor_tensor(out=out, in0=in0, scalar=scalar, in1=in1, op0=op0, op1=op1)
        self.S.dve(f, r=rr, w=[out])

    def cp(self, out, in_, eng="dve"):
        f = lambda e: e.tensor_copy(out=out, in_=in_)
        {"dve": self.S.dve, "pool": self.S.pool}[eng](f, r=[in_], w=[out])

    def memset(self, ap, v, eng="dve"):
        f = lambda e: e.memset(ap, v)
        {"dve": self.S.dve, "pool": self.S.pool}[eng](f, w=[ap])

    def rstd_of(self, src, nt, width, n_feat):
        T = self.T
        pb = T["ps"][2 + (self.cnt_norm % 2)]
        self.cnt_norm += 1
        for k in range(nt):
            sq = T["sq"][:, k % 2, 0:width]
            self.actf(sq, src[:, k, 0:width], AF.Square)
            self.mm(pb[:, 0:width], T["ones_b"][:], sq, start=(k == 0), stop=(k == nt - 1))
        self.actf(T["lnt"][:, 0:width], pb[:, 0:width], AF.Ln, scale=1.0 / n_feat, bias=T["eps"][:, 0:1])
        self.actf(T["rstd"][:, 0:width], T["lnt"][:, 0:width], AF.Exp, scale=-0.5)
        return T["rstd"]

    def pre_norm(self, li, j):
        T = self.T
        rstd = self.rstd_of(T["h"], KT, BLK, D)
        for k in range(KT):
            self.stt(T["hn"][:, k, :], T["h"][:, k, :], T["nw"][:, (li * 4 + j) * KT + k:(li * 4 + j) * KT + k + 1],
                     rstd[:, 0:BLK], ALU.mult, ALU.mult)

    def post_norm_add(self, li, j):
        T = self.T
        rstd = self.rstd_of(T["msb"], KT, BLK, D)
        for k in range(KT):
            tmp = T["tmp"][:, k % 2, :]
            self.stt(tmp, T["msb"][:, k, :], T["nw"][:, (li * 4 + j) * KT + k:(li * 4 + j) * KT + k + 1],
                     rstd[:, 0:BLK], ALU.mult, ALU.mult)
            self.tt(T["h"][:, k, :], T["h"][:, k, :], tmp, ALU.add, eng="pool")

    def wslot(self):
        w = self.T["wbuf"][self.cnt_w % 3]
        self.cnt_w += 1
        return w

    def ffn(self, li):
        T = self.T
        S = self.S
        self.pre_norm(li, 2)
        uT = T["arena"][:, 0:FT * BLK].rearrange("p (f t) -> p f t", t=BLK)
        wup = self.wb["w_up"][self.wi[li]].rearrange("(kt p) f -> p kt f", p=128)
        for fg in range(FT // 4):
            wt = self.wslot()
            wv = wt[:, 0:KT * 512].rearrange("p (k c) -> p k c", c=512)
            S.dma(wv, wup[:, :, fg * 512:(fg + 1) * 512])
            for f4 in range(4):
                f = fg * 4 + f4
                pb = T["ps"][f % 2]
                for k in range(KT):
                    self.mm(pb[:, 0:BLK], wv[:, k, f4 * 128:(f4 + 1) * 128], T["hn"][:, k, :], start=(k == 0), stop=(k == KT - 1))
                r = T["relu"][:, f % 2, :]
                self.actf(r, pb[:, 0:BLK], AF.Relu)
                self.tt(uT[:, f, :], r, pb[:, 0:BLK], ALU.mult)
        wdn = self.wb["w_down"][self.wi[li]].rearrange("(ft p) c -> p ft c", p=128)
        for o in range(KT):
            wt = self.wslot()
            wv = wt[:, 0:FT * 128].rearrange("p (f c) -> p f c", c=128)
            S.dma(wv, wdn[:, :, o * 128:(o + 1) * 128])
            pb = T["ps"][4 + (o % 2)]
            for f in range(FT):
                self.mm(pb[:, 0:BLK], wv[:, f, :], uT[:, f, :], start=(f == 0), stop=(f == FT - 1))
            self.actf(T["msb"][:, o, :], pb[:, 0:BLK], AF.Copy)
        self.post_norm_add(li, 3)

    def convert(self, name, idx, rows, cols, pieces=4):
        src = self.din[name][idx]
        dst = self.wb[name][idx]
        step = rows // pieces
        for i in range(pieces):
            self.S.dma(dst[i * step:(i + 1) * step, :], src[i * step:(i + 1) * step, :], q="pool")

    def build(self):
        import contextlib
        nc, S, L = self.nc, self.S, self.L
        NL = 4
        NW = self.nlw
        xT = self.inp("xT", [D, L])
        nw_d = self.inp("nw", [128, NL * 4 * KT])
        self.inp("w_up", [NW, D, DFF])
        self.inp("w_down", [NW, DFF, D])
        outT = nc.dram_tensor("outT", [D, L], F32, kind="ExternalOutput").ap()
        hbuf = self.scratch("hbuf", [self.NB, 128, KT * BLK], F32)
        self.wb = {
            "w_up": self.scratch("w_up_bf", [NW, D, DFF], BF16),
            "w_down": self.scratch("w_down_bf", [NW, DFF, D], BF16),
        }
        self.declare_mixer_inputs()
        self.cnt_norm = 0
        self.cnt_w = 0
        with contextlib.ExitStack() as st:
            sb = lambda n, s, d: st.enter_context(nc.sbuf_tensor(n, s, d))
            T = self.T = {}
            T["ps"] = [st.enter_context(nc.psum_tensor("ps%d" % i, [128, 512], F32)) for i in range(6)]
            T["psb"] = [st.enter_context(nc.psum_tensor("psb%d" % i, [128, 1024], BF16)) for i in range(2)]
            T["h"] = sb("h", [128, KT, BLK], F32)
            T["hn"] = sb("hn", [128, KT, BLK], BF16)
            T["msb"] = sb("msb", [128, KT, BLK], F32)
            T["sq"] = sb("sq", [128, 2, BLK], BF16)
            T["lnt"] = sb("lnt", [128, BLK], F32)
            T["rstd"] = sb("rstd", [128, BLK], F32)
            T["tmp"] = sb("tmp", [128, 2, BLK], F32)
            T["relu"] = sb("relu", [128, 2, BLK], F32)
            T["wbuf"] = [sb("wbuf%d" % i, [128, 4096], BF16) for i in range(3)]
            T["arena"] = sb("arena", [128, FT * BLK], BF16)
            T["nw"] = sb("nwt", [128, NL * 4 * KT], F32)
            T["ones_b"] = sb("ones_b", [128, 128], BF16)
            T["eps"] = sb("epst", [128, 1], F32)
            T["ones_f"] = sb("ones_f", [128, 128], F32)
            self.alloc_mixer_tiles(sb)
            S.dma(T["nw"][:], nw_d)
            self.memset(T["ones_b"][:], 1.0)
            self.memset(T["eps"][:], EPS)
            self.memset(T["ones_f"][:], 1.0)
            self.load_mixer_consts()
            for li in self.layers:
                self.convert_mixer_weights(li)
                self.convert("w_up", self.wi[li], D, DFF)
                self.convert("w_down", self.wi[li], DFF, D)
            xv = xT.rearrange("(kt p) t -> p kt t", p=128)
            ov = outT.rearrange("(kt p) t -> p kt t", p=128)
            for n, li in enumerate(self.layers):
                self.layer_begin(li)
                for b in range(self.NB):
                    hb = hbuf[b].rearrange("p (k t) -> p k t", t=BLK)
                    if n == 0:
                        S.dma(T["h"][:], xv[:, :, b * BLK:(b + 1) * BLK])
                    else:
                        S.dma(T["h"][:], hb)
                    if self.mixers:
                        self.mixer(li, b)
                    self.ffn(li)
                    if n == len(self.layers) - 1:
                        S.dma(ov[:, :, b * BLK:(b + 1) * BLK], T["h"][:])
                    else:
                        S.dma(hb, T["h"][:])
            S.emit()
        return nc

    MIXF = 16384
    MIXB = 31744

    def declare_mixer_inputs(self):
        odd = [li for li in self.layers if li % 2 == 1]
        even = [li for li in self.layers if li % 2 == 0]
        self.wi5 = {li: i for i, li in enumerate(odd)}
        self.wie = {li: i for i, li in enumerate(even)}
        self.inp("cn", [128, CN_N])
        if not self.mixers:
            return
        n5, ne = max(1, len(odd)), max(1, len(even))
        self.inp("s5pack", [n5, 128, PK_N])
        self.inp("w_glu_a", [n5, D, D])
        self.inp("w_glu_b", [n5, D, D])
        self.wb["w_glu_a"] = self.scratch("w_glu_a_bf", [n5, D, D], BF16)
        self.wb["w_glu_b"] = self.scratch("w_glu_b_bf", [n5, D, D], BF16)
        even_declare(self, ne)

    def alloc_mixer_tiles(self, sb):
        T = self.T
        T["cn"] = sb("cnt", [128, CN_N], F32)
        T["negpi"] = sb("negpi", [128, 1], F32)
        T["halfpi"] = sb("halfpi", [128, 1], F32)
        T["mixf"] = sb("mixf", [128, self.MIXF], F32)
        T["mixb"] = sb("mixb", [128, self.MIXB], BF16)
        self.cf = Carver(T["mixf"], self.MIXF)
        self.cb = Carver(T["mixb"], self.MIXB)

    def load_mixer_consts(self):
        self.S.dma(self.T["cn"][:], self.din["cn"])
        self.memset(self.T["negpi"][:], -PI)
        self.memset(self.T["halfpi"][:], PI / 2)

    def convert_mixer_weights(self, li):
        if not self.mixers:
            return
        if li % 2 == 1:
            self.convert("w_glu_a", self.wi5[li], D, D, pieces=2)
            self.convert("w_glu_b", self.wi5[li], D, D, pieces=2)
        else:
            even_convert(self, li)

    def layer_begin(self, li):
        if not self.mixers:
            return
        if getattr(self, "_open", None) is not None:
            self._open(self)
            self._open = None
        if li % 2 == 1:
            self.cf.mark(); self.cb.mark()
            s5_alloc(self)
            s5_prep(self, li)
            self._open = lambda P: (s5_end(P), P.cf.release(), P.cb.release())
        else:
            self.cf.mark(); self.cb.mark()
            even_begin(self, li)
            self._open = lambda P: (P.cf.release(), P.cb.release())

    def mixer(self, li, b):
        if li % 2 == 1:
            s5_mixer(self, li, b)
        else:
            even_mixer(self, li, b)


def even_declare(P, ne):
    P.inp("epack", [ne, 128, EP_N])
    P.inp("w_in", [ne, D, INP])
    P.inp("w_out", [ne, MIXW, D])
    P.inp("rot", [4, 128, P.L])
    P.wb["w_in"] = P.scratch("w_in_bf", [ne, D, INP], BF16)
    P.wb["w_out"] = P.scratch("w_out_bf", [ne, MIXW, D], BF16)


def even_convert(P, li):
    j = P.wie[li]
    P.convert("w_in", j, D, INP, pieces=8)
    P.convert("w_out", j, MIXW, D, pieces=4)


def even_begin(P, li):
    S, T = P.S, P.T
    cf, cb = P.cf, P.cb
    E = P.TE = {}
    j = P.wie[li]
    f = lambda n: cf.get(n)
    h = lambda n: cb.get(n)
    E["R"], E["S"], E["halo"], E["ep"] = f(2048), f(1024), f(36), f(EP_N)
    E["A"] = f(16)
    E["rot"] = f(4 * BLK)
    E["a1"], E["a2"] = f(2 * BLK), f(2 * BLK)
    E["rt"] = f(4 * BLK)
    E["xpre"] = f(2 * (BLK + 3))
    E["acc"] = f(2 * BLK)
    E["dt"], E["dA"], E["dtp"] = f(32), f(32), f(32)
    E["y_sb"], E["tA"], E["tB"] = f(1024), f(1024), f(1024)
    E["E"] = f(1024)
    E["CBTm"] = f(256)
    E["sm"] = f(128)
    E["Rb"], E["Sb"], E["idb"] = h(2048), h(1024), h(128)
    E["q"], E["k"], E["v"], E["sg"], E["sz"] = h(KT * BLK), h(KT * BLK), h(2 * 1024), h(2 * 1024), h(2 * 1024)
    E["xc"], E["ymf"] = h(12 * BLK), h(16 * BLK)
    E["wdt"] = h(KT * 16)
    E["PT"], E["kz"], E["xs"], E["X"], E["Xd"] = h(512), h(1024), h(1024), h(1024), h(1024)
    E["es"], E["MT"], E["ymt"], E["Bt"] = h(1024), h(1024), h(2048), h(256)
    S.dma(E["ep"], P.din["epack"][j])
    P.memset(E["R"], 0.0)
    P.memset(E["S"], 0.0)
    P.memset(E["halo"], 0.0)
    P.memset(E["Rb"], 0.0, eng="pool")
    P.memset(E["Sb"], 0.0, eng="pool")
    P.cp(E["idb"], T["cn"][:, CN_ID:CN_ID + 128])
    P.actf(E["A"], E["ep"][:, EP_ALOG:EP_ALOG + 16], AF.Exp)
    P.ts(E["A"], E["A"], -1.0, ALU.mult)
    S.dma(vap(E["wdt"], [(16, KT), (1, 16)]), P.wb["w_in"][j].rearrange("(kt p) f -> p kt f", p=128)[:, :, 6656:6672])


def even_mixer(P, li, b):
    S, T, E = P.S, P.T, P.TE
    j = P.wie[li]
    cn = T["cn"]
    hn = T["hn"]
    ps = T["ps"]
    P.pre_norm(li, 0)
    t0 = b * BLK
    S.dma(vap(E["rot"], [(BLK, 4), (1, BLK)]), P.din["rot"][:, :, t0:t0 + BLK].rearrange("f p t -> p f t"))
    rot = lambda i: E["rot"][:, i * BLK:(i + 1) * BLK]
    win = P.wb["w_in"][j].rearrange("(kt p) f -> p kt f", p=128)
    q3 = lambda X, t: X[:, t * BLK:(t + 1) * BLK]
    for cbk in range(13):
        wt = P.wslot()
        wv = wt[:, 0:KT * 512].rearrange("p (k c) -> p k c", c=512)
        S.dma(wv, win[:, :, cbk * 512:(cbk + 1) * 512])
        if cbk < 4:
            dst = E["q"] if cbk < 2 else E["k"]
            ci, si = (0, 1) if cbk < 2 else (2, 3)
            for hh in range(2):
                head = (cbk % 2) * 2 + hh
                pb1, pb2 = ps[2 * (hh % 2)], ps[2 * (hh % 2) + 1]
                for half, pb in ((0, pb1), (1, pb2)):
                    c0 = (hh * 2 + half) * 128
                    for k in range(KT):
                        P.mm(pb[:, 0:BLK], wv[:, k, c0:c0 + 128], hn[:, k, :], start=(k == 0), stop=(k == KT - 1))
                a1 = E["a1"][:, (hh % 2) * BLK:(hh % 2 + 1) * BLK]
                a2 = E["a2"][:, (hh % 2) * BLK:(hh % 2 + 1) * BLK]
                P.actf(a1, pb1[:, 0:BLK], AF.Copy)
                P.actf(a2, pb2[:, 0:BLK], AF.Copy)
                r1, r2, r3, r4 = (E["rt"][:, i * BLK:(i + 1) * BLK] for i in range(4))
                P.tt(r1, a1, rot(ci), ALU.mult)
                P.tt(r2, a2, rot(si), ALU.mult)
                P.tt(q3(dst, 2 * head), r1, r2, ALU.subtract)
                P.tt(r3, a2, rot(ci), ALU.mult, eng="pool")
                P.tt(r4, a1, rot(si), ALU.mult, eng="pool")
                P.tt(q3(dst, 2 * head + 1), r3, r4, ALU.add, eng="pool")
        elif cbk < 10:
            kind = (cbk - 4) // 2
            dst = (E["v"], E["sg"], E["sz"])[kind]
            fn = (AF.Copy, AF.Silu, AF.Silu)[kind]
            c0 = ((cbk - 4) % 2) * 512
            for c in range(2):
                pb = ps[4 + c]
                for k in range(KT):
                    P.mm(pb[:, 0:512], hn[:, k, c * 128:(c + 1) * 128], wv[:, k, :], start=(k == 0), stop=(k == KT - 1))
                P.actf(dst[:, c * 1024 + c0:c * 1024 + c0 + 512], pb[:, 0:512], fn)
        else:
            for f4 in range(4):
                t = (cbk - 10) * 4 + f4
                pb = ps[t % 4]
                for k in range(KT):
                    P.mm(pb[:, 0:BLK], wv[:, k, f4 * 128:(f4 + 1) * 128], hn[:, k, :], start=(k == 0), stop=(k == KT - 1))
                xp = E["xpre"][:, (t % 2) * (BLK + 3):(t % 2 + 1) * (BLK + 3)]
                acc = E["acc"][:, (t % 2) * BLK:(t % 2 + 1) * BLK]
                P.cp(xp[:, 0:3], E["halo"][:, t * 3:t * 3 + 3], eng="pool")
                P.actf(xp[:, 3:3 + BLK], pb[:, 0:BLK], AF.Copy)
                P.cp(E["halo"][:, t * 3:t * 3 + 3], xp[:, BLK:BLK + 3], eng="pool")
                cw = lambda jj: E["ep"][:, EP_CW + t * 4 + jj:EP_CW + t * 4 + jj + 1]
                P.ts(acc, xp[:, 0:BLK], cw(0), ALU.mult, E["ep"][:, EP_CB + t:EP_CB + t + 1], ALU.add)
                for jj in range(1, 4):
                    P.stt(acc, xp[:, jj:jj + BLK], cw(jj), acc, ALU.mult, ALU.add)
                P.actf(q3(E["xc"], t), acc, AF.Silu)
    for c in range(2):
        pb = ps[3][:, 416:448]
        for k in range(KT):
            P.mm(pb[:, c * 16:(c + 1) * 16], hn[:, k, c * 128:(c + 1) * 128], E["wdt"][:, k * 16:(k + 1) * 16], start=(k == 0), stop=(k == KT - 1))
        dtp = E["dtp"][:, c * 16:(c + 1) * 16]
        P.tt(dtp, pb[:, c * 16:(c + 1) * 16], E["ep"][:, EP_DTB:EP_DTB + 16], ALU.add)
        P.actf(dtp, dtp, AF.Exp)
        P.actf(E["dt"][:, c * 16:(c + 1) * 16], dtp, AF.Ln, bias=1.0)
        P.tt(E["dA"][:, c * 16:(c + 1) * 16], E["dt"][:, c * 16:(c + 1) * 16], E["A"], ALU.mult)
    import os
    STOP = float(os.environ.get("EVEN_STOP", "99"))
    if STOP <= 1:
        return
    mle = cn[:, CN_MLE:CN_MLE + 128]
    tgt = cn[:, CN_TGT:CN_TGT + 128]
    for c in range(2):
        cs = slice(c * 128, (c + 1) * 128)
        kf = lambda t: E["k"][:, t * BLK + c * 128:t * BLK + (c + 1) * 128]
        qf = lambda t: E["q"][:, t * BLK + c * 128:t * BLK + (c + 1) * 128]
        xcf = lambda t: E["xc"][:, t * BLK + c * 128:t * BLK + (c + 1) * 128]
        vt = lambda h: E["v"][:, c * 1024 + h * 256:c * 1024 + (h + 1) * 256]
        for h in range(4):
            for d2 in range(2):
                P.mm(ps[0][:, h * 128:(h + 1) * 128], kf(2 * h + d2), qf(2 * h + d2), start=(d2 == 0), stop=(d2 == 1))
        P.tt(E["PT"], ps[0][:, 0:512], cn[:, CN_DECAY:CN_DECAY + 512], ALU.mult)
        p1b = T["psb"][0][:, :]
        if STOP <= 1.2:
            continue
        for t in range(8):
            P.tr(p1b[:, t * 128:(t + 1) * 128], kf(t), E["idb"])
        for h in range(4):
            P.ts(E["kz"][:, h * 256:(h + 1) * 256], p1b[:, h * 256:(h + 1) * 256], cn[:, CN_ZETA + h:CN_ZETA + h + 1], ALU.mult)
        if STOP <= 1.4:
            continue
        yin = lambda h: ps[4 + h // 2][:, (h % 2) * 256:(h % 2 + 1) * 256]
        yx = lambda h: ps[1 + h // 2][:, (h % 2) * 256:(h % 2 + 1) * 256]
        for h in range(4):
            P.mm(yin(h), E["PT"][:, h * 128:(h + 1) * 128], vt(h))
        if STOP <= 1.6:
            continue
        for h in range(4):
            for d2 in range(2):
                P.mm(yx(h), qf(2 * h + d2), E["Rb"][:, (2 * h + d2) * 256:(2 * h + d2 + 1) * 256], start=(d2 == 0), stop=(d2 == 1))
        for h in range(4):
            P.ts(E["tA"][:, h * 256:(h + 1) * 256], yx(h), cn[:, CN_XI + h:CN_XI + h + 1], ALU.mult)
        for hb in range(2):
            P.tt(E["y_sb"][:, hb * 512:(hb + 1) * 512], ps[4 + hb][:, 0:512], E["tA"][:, hb * 512:(hb + 1) * 512], ALU.add)
        if STOP <= 1.8:
            continue
        for h in range(4):
            pb = ps[3]
            for d2 in range(2):
                P.mm(pb[:, d2 * 256:(d2 + 1) * 256], E["kz"][:, (2 * h + d2) * 128:(2 * h + d2 + 1) * 128], vt(h))
            Rh = E["R"][:, 2 * h * 256:(2 * h + 2) * 256]
            P.stt(Rh, Rh, RET_GCH[h], pb[:, 0:512], ALU.mult, ALU.add)
            P.cp(E["Rb"][:, 2 * h * 256:(2 * h + 2) * 256], Rh, eng="pool")
        if STOP <= 2:
            continue
        sm = E["sm"]
        y3 = vap(E["y_sb"], [(256, 4), (1, 256)])
        S.dve(lambda e, y3=y3: e.reduce_sum(out=sm[:, 0:4], in_=y3, axis=AX.X), r=[E["y_sb"]], w=[sm[:, 0:4]])
        P.tt(E["tB"], E["y_sb"], E["y_sb"], ALU.mult, eng="pool")
        t3 = vap(E["tB"], [(256, 4), (1, 256)])
        S.dve(lambda e, t3=t3: e.reduce_sum(out=sm[:, 4:8], in_=t3, axis=AX.X), r=[E["tB"]], w=[sm[:, 4:8]])
        P.ts(sm[:, 8:12], sm[:, 0:4], 1.0 / 256, ALU.mult)
        P.tt(sm[:, 12:16], sm[:, 8:12], sm[:, 8:12], ALU.mult)
        P.stt(sm[:, 16:20], sm[:, 4:8], 1.0 / 256, sm[:, 12:16], ALU.mult, ALU.subtract)
        P.actf(sm[:, 20:24], sm[:, 16:20], AF.Ln, bias=T["eps"][:, 0:1])
        P.actf(sm[:, 24:28], sm[:, 20:24], AF.Exp, scale=-0.5)
        for h in range(4):
            P.ts(E["tA"][:, h * 256:(h + 1) * 256], E["y_sb"][:, h * 256:(h + 1) * 256], sm[:, 8 + h:9 + h], ALU.subtract,
                 sm[:, 24 + h:25 + h], ALU.mult)
        P.tt(E["ymt"][:, 0:1024], E["tA"], E["sg"][:, c * 1024:(c + 1) * 1024], ALU.mult)
        if STOP <= 3:
            continue
        p2b = T["psb"][1][:, :]
        for t in range(8):
            P.tr(p2b[:, t * 128:(t + 1) * 128], xcf(t), E["idb"])
        P.actf(E["xs"], p2b, AF.Copy)
        dtc = E["dt"][:, c * 16:(c + 1) * 16]
        dAc = E["dA"][:, c * 16:(c + 1) * 16]
        hb3 = lambda a: vap(a, [(1, 16), (0, 64)])
        x3 = lambda a: vap(a, [(64, 16), (1, 64)])
        P.tt(x3(E["X"]), x3(E["xs"]), hb3(dtc), ALU.mult)
        P.mm(ps[3][:, 384:400], mle, dAc)
        P.mm(ps[3][:, 400:416], T["ones_f"][:], dAc)
        P.actf(sm[:, 32:48], ps[3][:, 384:400], AF.Copy)
        P.tt(sm[:, 48:64], ps[3][:, 400:416], sm[:, 32:48], ALU.subtract)
        P.actf(sm[:, 48:64], sm[:, 48:64], AF.Exp)
        P.actf(sm[:, 64:80], sm[:, 32:48], AF.Exp)
        P.actf(sm[:, 80:96], ps[3][:, 400:416], AF.Exp)
        P.tt(x3(E["Xd"]), x3(E["X"]), hb3(sm[:, 48:64]), ALU.mult)
        for g in range(2):
            P.mm(ps[3][:, g * 128:(g + 1) * 128], xcf(8 + g), xcf(10 + g))
        P.tt(vap(E["CBTm"], [(128, 2), (1, 128)]), vap(ps[3][:, 0:256], [(128, 2), (1, 128)]), vap(mle, [(0, 2), (1, 128)]), ALU.mult)
        p3b = p1b[:, 0:256]
        for g in range(2):
            P.tr(p3b[:, g * 128:(g + 1) * 128], xcf(8 + g), E["idb"])
        P.actf(E["Bt"], p3b, AF.Copy)
        for g in range(2):
            P.tt(vap(E["E"], [(128, 8), (1, 128)]), vap(mle, [(0, 8), (1, 128)]), vap(dAc[:, g * 8:], [(1, 8), (0, 128)]), ALU.mult)
            for hf in range(2):
                pb = (ps[4], ps[5], ps[1], ps[2])[2 * g + hf]
                P.mm(pb[:, 0:512], tgt, E["E"][:, hf * 512:(hf + 1) * 512])
                P.actf(E["es"][:, hf * 512:(hf + 1) * 512], pb[:, 0:512], AF.Exp)
            P.tt(vap(E["MT"], [(128, 8), (1, 128)]), vap(E["es"], [(128, 8), (1, 128)]),
                 vap(E["CBTm"][:, g * 128:], [(0, 8), (1, 128)]), ALU.mult, eng="pool")
            for h8 in range(8):
                hh = g * 8 + h8
                P.mm(ps[(0, 3)[g]][:, h8 * 64:(h8 + 1) * 64], E["MT"][:, h8 * 128:(h8 + 1) * 128], E["X"][:, hh * 64:(hh + 1) * 64])
        if STOP <= 4:
            continue
        for g in range(2):
            P.mm(ps[4 + g][:, 0:512], xcf(10 + g), E["Sb"][:, g * 512:(g + 1) * 512])
        for g in range(2):
            P.mm(ps[1 + g][:, 0:512], E["Bt"][:, g * 128:(g + 1) * 128], E["Xd"][:, g * 512:(g + 1) * 512])
        h8v = lambda a: vap(a, [(64, 8), (1, 64)])
        for g in range(2):
            sl = slice(g * 512, (g + 1) * 512)
            P.tt(h8v(E["tA"][:, sl]), h8v(ps[4 + g][:, 0:512]), vap(sm[:, 64 + g * 8:], [(1, 8), (0, 64)]), ALU.mult)
            P.tt(E["tB"][:, sl], ps[(0, 3)[g]][:, 0:512], E["tA"][:, sl], ALU.add)
        P.tt(x3(E["tA"]), x3(E["xs"]), hb3(E["ep"][:, EP_D:EP_D + 16]), ALU.mult, eng="pool")
        P.tt(E["tB"], E["tB"], E["tA"], ALU.add, eng="pool")
        P.tt(x3(E["S"]), x3(E["S"]), hb3(sm[:, 80:96]), ALU.mult)
        for g in range(2):
            sl = slice(g * 512, (g + 1) * 512)
            P.tt(E["S"][:, sl], E["S"][:, sl], ps[1 + g][:, 0:512], ALU.add)
        P.cp(E["Sb"], E["S"], eng="pool")
        P.tt(E["tB"], E["tB"], E["sz"][:, c * 1024:(c + 1) * 1024], ALU.mult)
        P.tt(E["tA"], E["tB"], E["tB"], ALU.mult, eng="pool")
        tg = vap(E["tA"], [(512, 2), (1, 512)])
        S.dve(lambda e, tg=tg: e.reduce_sum(out=sm[:, 96:98], in_=tg, axis=AX.X), r=[E["tA"]], w=[sm[:, 96:98]])
        P.actf(sm[:, 98:100], sm[:, 96:98], AF.Ln, scale=1.0 / 512, bias=T["eps"][:, 0:1])
        P.actf(sm[:, 100:102], sm[:, 98:100], AF.Exp, scale=-0.5)
        for g in range(2):
            P.ts(E["ymt"][:, 1024 + g * 512:1024 + (g + 1) * 512], E["tB"][:, g * 512:(g + 1) * 512], sm[:, 100 + g:101 + g], ALU.mult)
        if STOP <= 5:
            continue
        for half in range(2):
            pbb = p1b if half == 0 else p2b
            for t8 in range(8):
                t = half * 8 + t8
                P.tr(pbb[:, t8 * 128:(t8 + 1) * 128], E["ymt"][:, t * 128:(t + 1) * 128], E["idb"])
            if half == 0:
                P.actf(vap(E["ymf"][:, c * 128:], [(BLK, 8), (1, 128)]), pbb.rearrange("p (t n) -> p t n", n=128), AF.Copy)
            else:
                for t8 in range(8):
                    t = 8 + t8
                    P.ts(E["ymf"][:, t * BLK + c * 128:t * BLK + (c + 1) * 128], pbb[:, t8 * 128:(t8 + 1) * 128],
                         E["ep"][:, EP_SN + t8:EP_SN + t8 + 1], ALU.mult)
    wo = P.wb["w_out"][j].rearrange("(kt p) c -> p kt c", p=128)
    for o in range(KT):
        wt = P.wslot()
        wv = wt[:, 0:16 * 128].rearrange("p (k c) -> p k c", c=128)
        S.dma(wv, wo[:, :, o * 128:(o + 1) * 128])
        pb = ps[4 + (o % 2)]
        for k in range(16):
            P.mm(pb[:, 0:BLK], wv[:, k, :], E["ymf"][:, k * BLK:(k + 1) * BLK], start=(k == 0), stop=(k == 15))
        P.actf(T["msb"][:, o, :], pb[:, 0:BLK], AF.Copy)
    P.post_norm_add(li, 1)


TC = 4
NS = BLK // TC
PI = float(np.pi)
PK_WLR, PK_WLI, PK_WLS, PK_WBR, PK_WBI = 0, 512, 1024, 1536, 2048
PK_VLR, PK_VLI, PK_VLS, PK_VCR, PK_VCI = 2560, 2592, 2624, 2656, 3168
PK_PLR, PK_PLI, PK_PLS, PK_PBR, PK_PBI, PK_PCR, PK_PCI = 3680, 4704, 5728, 6752, 7776, 8800, 9824
PK_D = 10848
PK_N = 10856
CN_MASKQ, CN_PAR, CN_BD, CN_IOTA = 0, 4, 6, 134
CN_DECAY = CN_IOTA + NS
CN_ZETA = CN_DECAY + 512
CN_XI = CN_ZETA + 4
CN_MLE = CN_XI + 4
CN_TGT = CN_MLE + 128
CN_ID = CN_TGT + 128
CN_N = CN_ID + 128
RET_LOGG = [float(np.log1p(-(2.0 ** (-5.0 - h)))) for h in range(4)]
RET_GCH = [float(np.exp(128.0 * lg)) for lg in RET_LOGG]
EP_CW, EP_CB, EP_DTB, EP_ALOG, EP_D, EP_SN = 0, 48, 60, 76, 92, 108
EP_N = 116


def vap(base, dims):
    return bass.AP(base.tensor, base.offset, [list(base.ap[0])] + [[s, c] for s, c in dims])


class Carver:
    def __init__(self, tile, ncols):
        self.t = tile
        self.n = ncols
        self.p = 0
        self.marks = []

    def get(self, ncols):
        a = self.p
        self.p += ncols
        assert self.p <= self.n, ("arena overflow", self.p, self.n)
        return self.t[:, a:a + ncols]

    def mark(self):
        self.marks.append(self.p)

    def release(self):
        self.p = self.marks.pop()


MAGIC = 12582912.0
I32 = mybir.dt.int32
LN2_HI = 0.693359375
LN2_LO = -2.12194440e-4
TWOPI_HI = 6.28125
TWOPI_LO = 0.0019353071795864769


def acc_exp(P, cf, out, x, w):
    cf.mark()
    kf, r, acc = cf.get(w), cf.get(w), cf.get(w)
    P.ts(kf, x, 1.4426950408889634, ALU.mult, MAGIC, ALU.add)
    P.ts(kf, kf, MAGIC, ALU.subtract)
    P.stt(r, kf, -LN2_HI, x, ALU.mult, ALU.add)
    P.stt(r, kf, -LN2_LO, r, ALU.mult, ALU.add)
    cs = [1.0 / 40320, 1.0 / 5040, 1.0 / 720, 1.0 / 120, 1.0 / 24, 1.0 / 6, 0.5, 1.0]
    P.ts(acc, r, cs[0], ALU.mult)
    for c in cs[1:]:
        P.stt(acc, acc, float(c), r, ALU.add, ALU.mult)
    P.ts(acc, acc, 1.0, ALU.add)
    ki = kf.bitcast(I32)
    P.cp(ki, kf)
    P.ts(ki, ki, 127, ALU.add)
    P.ts(ki, ki, 23, ALU.logical_shift_left)
    P.tt(out, acc, ki.bitcast(F32), ALU.mult)
    cf.release()


def acc_sincos(P, cf, s_out, c_out, ang, w):
    cf.mark()
    kf, r = cf.get(w), cf.get(w)
    for out, turns, bias in ((s_out, 0.0, None), (c_out, 0.25, P.T["halfpi"][:, 0:1])):
        P.ts(kf, ang, 1.0 / (2 * PI), ALU.mult, float(turns), ALU.add)
        P.ts(kf, kf, MAGIC, ALU.add)
        P.ts(kf, kf, MAGIC, ALU.subtract)
        P.stt(r, kf, -TWOPI_HI, ang, ALU.mult, ALU.add)
        P.stt(r, kf, -TWOPI_LO, r, ALU.mult, ALU.add)
        if bias is None:
            P.actf(out, r, AF.Sin)
        else:
            P.actf(out, r, AF.Sin, bias=bias)
    cf.release()


def s5_coefs(P, cf, lr_raw, li, ls, w, kmax):
    g = lambda: cf.get(w)
    lr, dl, ar, ai = g(), g(), g(), g()
    P.ts(lr, lr_raw, -1e-4, ALU.min)
    acc_exp(P, cf, dl, ls, w)
    P.tt(ar, lr, dl, ALU.mult)
    P.tt(ai, li, dl, ALU.mult)
    pr, pi, ur, ui = {}, {}, {}, {}
    mag, t1, t2, tmp = g(), g(), g(), g()
    ur[1], ui[1], pr[1], pi[1] = g(), g(), g(), g()
    acc_exp(P, cf, mag, ar, w)
    acc_sincos(P, cf, ui[1], ur[1], ai, w)
    P.tt(pr[1], mag, ur[1], ALU.mult)
    P.tt(pi[1], mag, ui[1], ALU.mult)
    for k in range(2, kmax + 1):
        pr[k], pi[k] = g(), g()
        cmul(P, pr[k], pi[k], pr[k - 1], pi[k - 1], pr[1], pi[1], t1, t2)
    nr, den = g(), g()
    cr, ci = g(), g()
    P.ts(nr, pr[1], -1.0, ALU.add)
    P.tt(den, lr, lr, ALU.mult)
    P.tt(t1, li, li, ALU.mult)
    P.tt(den, den, t1, ALU.add)
    P.S.dve(lambda e: e.reciprocal(out=den, in_=den), r=[den], w=[den])
    P.tt(t1, nr, lr, ALU.mult)
    P.tt(t2, pi[1], li, ALU.mult)
    P.tt(t1, t1, t2, ALU.add)
    P.tt(cr, t1, den, ALU.mult)
    P.tt(t1, pi[1], lr, ALU.mult)
    P.tt(t2, nr, li, ALU.mult)
    P.tt(t1, t1, t2, ALU.subtract)
    P.tt(ci, t1, den, ALU.mult)
    return dict(pr=pr, pi=pi, ur=ur, ui=ui, cr=cr, ci=ci, t1=t1, t2=t2, ar=ar, ai=ai, tmp=tmp, mag=mag)


def cmul(P, outr, outi, ar, ai, br, bi, t1, t2):
    P.tt(t1, ar, br, ALU.mult)
    P.tt(t2, ai, bi, ALU.mult)
    P.tt(outr, t1, t2, ALU.subtract)
    P.tt(t1, ar, bi, ALU.mult)
    P.tt(t2, ai, br, ALU.mult)
    P.tt(outi, t1, t2, ALU.add)


def s5_alloc(P):
    T = P.T
    cf, cb = P.cf, P.cb
    T5 = P.T5 = {}
    T5["Wt"] = cb.get(KT * TC * 2 * 128)
    T5["Vt"] = cb.get(32 * TC * 2 * 32)
    T5["Kb"] = cb.get(KT * TC * 128)
    T5["cosT"] = cf.get(32 * NS)
    T5["sinT"] = cf.get(32 * NS)
    T5["Amul"] = cf.get(32 * NS)
    T5["mur"] = cf.get(32)
    T5["mui"] = cf.get(32)
    T5["xpr"] = cf.get(32)
    T5["xpi"] = cf.get(32)
    T5["ds5"] = cf.get(KT)
    T5["nwm"] = cf.get(4 * KT)


def s5_prep(P, li):
    S, T, T5 = P.S, P.T, P.T5
    cf, cb = P.cf, P.cb
    j = P.wi5[li]
    pk = P.din["s5pack"][j]
    cn = T["cn"]
    cf.mark()
    cb.mark()
    def ld(col, w):
        t = cf.get(w)
        S.dma(t, pk[:, col:col + w])
        return t
    Wt = T5["Wt"]
    P.memset(Wt, 0.0)
    for half in range(2):
        cf.mark()
        w = 256
        o5 = half * 256
        lr, lim, ls = ld(PK_WLR + o5, w), ld(PK_WLI + o5, w), ld(PK_WLS + o5, w)
        br, bi = ld(PK_WBR + o5, w), ld(PK_WBI + o5, w)
        c = s5_coefs(P, cf, lr, lim, ls, w, TC - 1)
        bbr, bbi, wr, wi_ = cf.get(w), cf.get(w), cf.get(w), cf.get(w)
        cmul(P, bbr, bbi, c["cr"], c["ci"], br, bi, c["t1"], c["t2"])
        for i in range(TC):
            k = TC - 1 - i
            if k == 0:
                sr, si = bbr, bbi
            else:
                cmul(P, wr, wi_, c["pr"][k], c["pi"][k], bbr, bbi, c["t1"], c["t2"])
                sr, si = wr, wi_
            for ri, src in ((0, sr), (1, si)):
                for par in range(2):
                    o = vap(Wt[:, (half * 4 * TC * 2 + i * 2 + ri) * 128 + par * 64:], [(TC * 2 * 128, 4), (1, 64)])
                    P.ts(o, vap(src, [(64, 4), (1, 64)]), cn[:, CN_PAR + par:CN_PAR + par + 1], ALU.mult)
        cf.release()
    cf.mark()
    w = 32
    lr, lim, ls = ld(PK_VLR, w), ld(PK_VLI, w), ld(PK_VLS, w)
    vcr, vci = ld(PK_VCR, 512), ld(PK_VCI, 512)
    c = s5_coefs(P, cf, lr, lim, ls, w, TC)
    P.cp(T5["mur"], c["pr"][TC])
    P.cp(T5["mui"], c["pi"][TC])
    Vt = T5["Vt"]
    P.memset(Vt, 0.0)
    vr, vi = cf.get(512), cf.get(512)
    t1, t2 = cf.get(512), cf.get(512)
    b3 = lambda a: vap(a, [(1, 32), (0, 16)])
    v3 = lambda a: vap(a, [(16, 32), (1, 16)])
    for jj in range(TC):
        k = jj + 1
        cmul(P, v3(vr), v3(vi), b3(c["pr"][k]), b3(c["pi"][k]), v3(vcr), v3(vci), v3(t1), v3(t2))
        for g2 in range(2):
            rows = slice(64 * g2, 64 * g2 + 64)
            o_r = vap(Vt[rows, (jj * 2 + 0) * 32 + g2 * 16:], [(TC * 2 * 32, 32), (1, 16)])
            o_i = vap(Vt[rows, (jj * 2 + 1) * 32 + g2 * 16:], [(TC * 2 * 32, 32), (1, 16)])
            P.cp(o_r, v3(vr[rows, :]))
            P.ts(o_i, v3(vi[rows, :]), -1.0, ALU.mult)
    amag, tca = cf.get(32), cf.get(32)
    P.ts(tca, c["ar"], float(TC), ALU.mult)
    acc_exp(P, cf, amag, tca, 32)
    mr, mi, m2r, m2i = cf.get(32), cf.get(32), cf.get(32), cf.get(32)
    P.cp(mr, c["ur"][1]); P.cp(mi, c["ui"][1])
    tt1, tt2 = cf.get(32), cf.get(32)
    k = 1
    while k < TC:
        cmul(P, m2r, m2i, mr, mi, mr, mi, tt1, tt2)
        P.cp(mr, m2r); P.cp(mi, m2i)
        k *= 2
    cosT, sinT = T5["cosT"], T5["sinT"]
    e3 = lambda a, lo, n: vap(a[:, lo:], [(NS, 32), (1, n)])
    bq = lambda a, n: vap(a, [(1, 32), (0, n)])
    P.memset(e3(cosT, 0, 1), 1.0)
    P.memset(e3(sinT, 0, 1), 0.0)
    sc1, sc2 = cf.get(32 * NS // 2), cf.get(32 * NS // 2)
    s = 1
    while s < NS:
        s3 = lambda a: vap(a, [(s, 32), (1, s)])
        cmul(P, e3(cosT, s, s), e3(sinT, s, s), e3(cosT, 0, s), e3(sinT, 0, s), bq(mr, s), bq(mi, s), s3(sc1), s3(sc2))
        cmul(P, m2r, m2i, mr, mi, mr, mi, tt1, tt2)
        P.cp(mr, m2r); P.cp(mi, m2i)
        s *= 2
    a3 = lambda a: vap(a, [(NS, 32), (1, NS)])
    P.cp(a3(T5["Amul"]), vap(amag, [(1, 32), (0, NS)]))
    P.memset(vap(T5["Amul"], [(NS, 32), (1, 1)]), 0.0)
    cf.release()
    Kb = T5["Kb"]
    for qd in range(4):
        cf.mark()
        w = 256
        o5 = qd * 256
        lr, lim, ls = ld(PK_PLR + o5, w), ld(PK_PLI + o5, w), ld(PK_PLS + o5, w)
        br, bi = ld(PK_PBR + o5, w), ld(PK_PBI + o5, w)
        pcr, pci = ld(PK_PCR + o5, w), ld(PK_PCI + o5, w)
        c = s5_coefs(P, cf, lr, lim, ls, w, TC - 1)
        bbr, bbi, ar_, ai_ = cf.get(w), cf.get(w), cf.get(w), cf.get(w)
        cmul(P, bbr, bbi, c["cr"], c["ci"], br, bi, c["t1"], c["t2"])
        for tau in range(TC):
            if tau == 0:
                sr, si = bbr, bbi
            else:
                cmul(P, ar_, ai_, c["pr"][tau], c["pi"][tau], bbr, bbi, c["t1"], c["t2"])
                sr, si = ar_, ai_
            nsi = c["tmp"]
            P.ts(nsi, si, -1.0, ALU.mult)
            for f2 in range(2):
                ft = qd * 2 + f2
                pb = T["ps"][4 + (f2 % 2)]
                P.mm(pb[:, 0:128], sr[:, f2 * 128:(f2 + 1) * 128], pcr[:, f2 * 128:(f2 + 1) * 128], start=True, stop=False)
                P.mm(pb[:, 0:128], nsi[:, f2 * 128:(f2 + 1) * 128], pci[:, f2 * 128:(f2 + 1) * 128], start=False, stop=True)
                P.tt(Kb[:, (ft * TC + tau) * 128:(ft * TC + tau + 1) * 128], pb[:, 0:128], cn[:, CN_BD:CN_BD + 128], ALU.mult)
        cf.release()
    P.dbg("Wt", T5["Wt"], BF16); P.dbg("Vt", T5["Vt"], BF16); P.dbg("Kb", T5["Kb"], BF16)
    P.dbg("cosT", T5["cosT"]); P.dbg("sinT", T5["sinT"]); P.dbg("Amul", T5["Amul"]); P.dbg("mur", T5["mur"]); P.dbg("mui", T5["mui"])
    S.dma(T5["ds5"], pk[:, PK_D:PK_D + KT])
    for q4 in range(4):
        P.ts(T5["nwm"][:, q4 * KT:(q4 + 1) * KT], T["nw"][:, (li * 4 + 0) * KT:(li * 4 + 0) * KT + KT],
             cn[:, CN_MASKQ + q4:CN_MASKQ + q4 + 1], ALU.mult)
    P.memset(T5["xpr"], 0.0)
    P.memset(T5["xpi"], 0.0)
    cf.release()
    cb.release()
    cf.mark()
    cb.mark()
    T5["Whr"] = cf.get(32 * NS)
    T5["Whi"] = cf.get(32 * NS)
    T5["t1"] = cf.get(512)
    T5["t2"] = cf.get(512)
    T5["ysum"] = cf.get(2 * BLK)
    T5["s1"] = cf.get(32)
    T5["s2"] = cf.get(32)
    T5["Xsr"] = cb.get(32 * (NS + 1))
    T5["Xsi"] = cb.get(32 * (NS + 1))
    T5["yg"] = cb.get(KT * BLK)
    T5["sig"] = cf.get(2 * BLK)


def s5_end(P):
    P.cf.release()
    P.cb.release()


def s5_mixer(P, li, b):
    S, T, T5 = P.S, P.T, P.T5
    j = P.wi5[li]
    hn = T["hn"]
    hm = T["arena"][:, 0:32 * BLK].rearrange("p (q t) -> p q t", t=BLK)
    rstd = P.rstd_of(T["h"], KT, BLK, D)
    for k in range(KT):
        P.stt(hn[:, k, :], T["h"][:, k, :], T["nw"][:, (li * 4) * KT + k:(li * 4) * KT + k + 1], rstd[:, 0:BLK], ALU.mult, ALU.mult)
        for q4 in range(4):
            P.stt(hm[:, k * 4 + q4, :], T["h"][:, k, :], T5["nwm"][:, q4 * KT + k:q4 * KT + k + 1], rstd[:, 0:BLK],
                  ALU.mult, ALU.mult, eng=("pool" if q4 % 2 else "dve"))
    Wt, Vt, Kb = T5["Wt"], T5["Vt"], T5["Kb"]
    Whr, Whi = T5["Whr"], T5["Whi"]
    for g in range(4):
        pr_, pi_ = T["ps"][0], T["ps"][1]
        for q8 in range(8):
            q = g * 8 + q8
            ft = q // 4
            for ri, pb in ((0, pr_), (1, pi_)):
                for i in range(TC):
                    lhsT = Wt[:, ((ft * TC + i) * 2 + ri) * 128:((ft * TC + i) * 2 + ri + 1) * 128]
                    rhs = vap(hm[:, q, i:], [(TC, NS)])
                    P.mm(pb[:, q8 * NS:(q8 + 1) * NS], lhsT, rhs, start=(i == 0), stop=(i == TC - 1))
        sl = slice(g * 512, (g + 1) * 512)
        cs_, sn_ = T5["cosT"][:, sl], T5["sinT"][:, sl]
        t1, t2 = T5["t1"], T5["t2"]
        P.tt(t1, pr_[:, 0:512], cs_, ALU.mult)
        P.tt(t2, pi_[:, 0:512], sn_, ALU.mult)
        P.tt(Whr[:, sl], t1, t2, ALU.add, eng="pool")
        P.tt(t1, pi_[:, 0:512], cs_, ALU.mult)
        P.tt(t2, pr_[:, 0:512], sn_, ALU.mult)
        P.tt(Whi[:, sl], t1, t2, ALU.subtract, eng="pool")
    s1, s2 = T5["s1"], T5["s2"]
    first = lambda a: vap(a, [(NS, 32)])
    P.tt(s1, T5["mur"], T5["xpr"], ALU.mult)
    P.tt(s2, T5["mui"], T5["xpi"], ALU.mult)
    P.tt(s1, s1, s2, ALU.subtract)
    P.tt(first(Whr), first(Whr), s1, ALU.add)
    P.tt(s1, T5["mur"], T5["xpi"], ALU.mult)
    P.tt(s2, T5["mui"], T5["xpr"], ALU.mult)
    P.tt(s1, s1, s2, ALU.add)
    P.tt(first(Whi), first(Whi), s1, ALU.add)
    for W_ in (Whr, Whi):
        S.dve(lambda e, W_=W_: e.tensor_tensor_scan(out=W_, data0=T5["Amul"], data1=W_, initial=0.0, op0=ALU.mult, op1=ALU.add),
              r=[T5["Amul"], W_], w=[W_])
    Xsr, Xsi = T5["Xsr"], T5["Xsi"]
    P.cp(vap(Xsr, [(NS + 1, 32)]), T5["xpr"])
    P.cp(vap(Xsi, [(NS + 1, 32)]), T5["xpi"])
    for g in range(4):
        sl = slice(g * 512, (g + 1) * 512)
        cs_, sn_ = T5["cosT"][:, sl], T5["sinT"][:, sl]
        t1, t2 = T5["t1"], T5["t2"]
        o3 = lambda X: vap(X[:, g * 8 * (NS + 1) + 1:], [(NS + 1, 8), (1, NS)])
        i3 = lambda a: vap(a, [(NS, 8), (1, NS)])
        P.tt(t1, Whr[:, sl], cs_, ALU.mult)
        P.tt(t2, Whi[:, sl], sn_, ALU.mult)
        P.tt(o3(Xsr), i3(t1), i3(t2), ALU.subtract, eng="pool")
        P.tt(vap(T5["xpr"][:, g * 8:], [(1, 8)]), vap(t1[:, NS - 1:], [(NS, 8)]), vap(t2[:, NS - 1:], [(NS, 8)]), ALU.subtract)
        P.tt(t1, Whi[:, sl], cs_, ALU.mult)
        P.tt(t2, Whr[:, sl], sn_, ALU.mult)
        P.tt(o3(Xsi), i3(t1), i3(t2), ALU.add, eng="pool")
        P.tt(vap(T5["xpi"][:, g * 8:], [(1, 8)]), vap(t1[:, NS - 1:], [(NS, 8)]), vap(t2[:, NS - 1:], [(NS, 8)]), ALU.add)
    yg = T5["yg"]
    for ft in range(KT):
        pb = T["ps"][4 + (ft % 2)]
        for jj in range(TC):
            ocol = vap(pb[:, jj:], [(TC, NS)])
            for tau in range(jj + 1):
                P.mm(ocol, Kb[:, (ft * TC + tau) * 128:(ft * TC + tau + 1) * 128], vap(hn[:, ft, jj - tau:], [(TC, NS)]),
                     start=(tau == 0), stop=False)
            for q4 in range(4):
                q = ft * 4 + q4
                kw = dict(tile_position=(0, 96)) if q4 == 3 else {}
                osub = vap(pb[32 * q4:32 * q4 + 32, jj:], [(TC, NS)])
                P.mm(osub, Vt[:, ((q * TC + jj) * 2 + 0) * 32:((q * TC + jj) * 2 + 1) * 32], Xsr[:, q * (NS + 1):q * (NS + 1) + NS],
                     start=False, stop=False, **kw)
                P.mm(osub, Vt[:, ((q * TC + jj) * 2 + 1) * 32:((q * TC + jj) * 2 + 2) * 32], Xsi[:, q * (NS + 1):q * (NS + 1) + NS],
                     start=False, stop=(q4 == 3), **kw)
        ys = T5["ysum"][:, (ft % 2) * BLK:(ft % 2 + 1) * BLK]
        P.stt(ys, hn[:, ft, :], T5["ds5"][:, ft:ft + 1], pb[:, 0:BLK], ALU.mult, ALU.add)
        P.actf(yg[:, ft * BLK:(ft + 1) * BLK], ys, AF.Gelu)
    wa = P.wb["w_glu_a"][j].rearrange("(kt p) c -> p kt c", p=128)
    wbq = P.wb["w_glu_b"][j].rearrange("(kt p) c -> p kt c", p=128)
    for o in range(KT):
        wt = P.wslot()
        wva = wt[:, 0:KT * 128].rearrange("p (k c) -> p k c", c=128)
        wvb = wt[:, KT * 128:2 * KT * 128].rearrange("p (k c) -> p k c", c=128)
        S.dma(wva, wa[:, :, o * 128:(o + 1) * 128])
        S.dma(wvb, wbq[:, :, o * 128:(o + 1) * 128])
        pa, pb2 = T["ps"][0], T["ps"][1]
        if o % 2:
            pa, pb2 = T["ps"][2], T["ps"][3]
        for k in range(KT):
            P.mm(pa[:, 0:BLK], wva[:, k, :], yg[:, k * BLK:(k + 1) * BLK], start=(k == 0), stop=(k == KT - 1))
        for k in range(KT):
            P.mm(pb2[:, 0:BLK], wvb[:, k, :], yg[:, k * BLK:(k + 1) * BLK], start=(k == 0), stop=(k == KT - 1))
        sg = T5["sig"][:, (o % 2) * BLK:(o % 2 + 1) * BLK]
        P.actf(sg, pb2[:, 0:BLK], AF.Sigmoid)
        P.tt(T["msb"][:, o, :], pa[:, 0:BLK], sg, ALU.mult)
    P.post_norm_add(li, 1)


def host_consts():
    cn = np.zeros((128, CN_N), np.float32)
    p = np.arange(128)
    for q4 in range(4):
        cn[:, CN_MASKQ + q4] = (p // 32 == q4)
    cn[:, CN_PAR + 0] = ((p // 16) % 2 == 0)
    cn[:, CN_PAR + 1] = ((p // 16) % 2 == 1)
    cn[:, CN_BD:CN_BD + 128] = (p[:, None] // 16 == p[None, :] // 16)
    cn[:, CN_IOTA:CN_IOTA + NS] = np.arange(NS, dtype=np.float32)[None, :]
    idx = np.arange(128, dtype=np.float64)
    rel = idx[None, :] - idx[:, None]
    for h in range(4):
        lg = RET_LOGG[h]
        cn[:, CN_DECAY + h * 128:CN_DECAY + (h + 1) * 128] = np.where(rel >= 0, np.exp(np.maximum(rel, 0) * lg), 0.0)
        cn[:, CN_ZETA + h] = np.exp((127.0 - idx) * lg)
        cn[:, CN_XI + h] = np.exp((idx + 1.0) * lg)
    cn[:, CN_MLE:CN_MLE + 128] = (p[:, None] <= p[None, :])
    cn[:, CN_TGT:CN_TGT + 128] = (p[:, None] > p[None, :])
    cn[:, CN_ID:CN_ID + 128] = np.eye(128)
    return cn


def host_rot(L):
    half = 128
    inv = (np.float32(10000.0) ** (-np.arange(half, dtype=np.float32) / np.float32(half))).astype(np.float32)
    ang = (np.arange(L, dtype=np.float32)[:, None] * inv[None, :]).astype(np.float32)
    c = np.cos(ang).astype(np.float32).T
    sn = np.sin(ang).astype(np.float32).T
    return np.ascontiguousarray(np.stack([c, sn, c / 16.0, sn / 16.0]).astype(np.float32))


def host_epack(conv_w, conv_b, dt_bias, a_log, d_ssd, ssd_norm):
    ep = np.zeros((128, EP_N), np.float32)
    ep[:, EP_CW:EP_CW + 48] = conv_w.reshape(4, 12, 128).transpose(2, 1, 0).reshape(128, 48)
    ep[:, EP_CB:EP_CB + 12] = conv_b.reshape(12, 128).T
    ep[:, EP_DTB:EP_DTB + 16] = np.broadcast_to(dt_bias[None, :], (128, 16))
    ep[:, EP_ALOG:EP_ALOG + 16] = np.broadcast_to(a_log[None, :], (128, 16))
    ep[:, EP_D:EP_D + 16] = np.broadcast_to(d_ssd[None, :], (128, 16))
    ep[:, EP_SN:EP_SN + 8] = ssd_norm.reshape(8, 128).T
    return ep


def host_s5pack(lam_re, lam_im, log_step, b_re, b_im, c_re, c_im, d_s5):
    pk = np.zeros((128, PK_N), np.float32)
    ls_gp = np.broadcast_to(log_step[:, None], (64, 64))
    def wl(a_gp):
        a = a_gp.reshape(8, 8, 64)
        a = np.broadcast_to(a[:, :, None, :], (8, 8, 16, 64))
        return a.transpose(1, 2, 0, 3).reshape(128, 512)
    def wb(b_gpc):
        a = b_gpc.reshape(8, 8, 64, 16)
        return a.transpose(1, 3, 0, 2).reshape(128, 512)
    pk[:, PK_WLR:PK_WLR + 512] = wl(lam_re)
    pk[:, PK_WLI:PK_WLI + 512] = wl(lam_im)
    pk[:, PK_WLS:PK_WLS + 512] = wl(ls_gp)
    pk[:, PK_WBR:PK_WBR + 512] = wb(b_re)
    pk[:, PK_WBI:PK_WBI + 512] = wb(b_im)
    def vl(a_gp):
        return a_gp.reshape(32, 2, 64).transpose(1, 2, 0).reshape(128, 32)
    def vc(c_gcp):
        return c_gcp.reshape(32, 2, 16, 64).transpose(1, 3, 0, 2).reshape(128, 512)
    pk[:, PK_VLR:PK_VLR + 32] = vl(lam_re)
    pk[:, PK_VLI:PK_VLI + 32] = vl(lam_im)
    pk[:, PK_VLS:PK_VLS + 32] = vl(ls_gp)
    pk[:, PK_VCR:PK_VCR + 512] = vc(c_re)
    pk[:, PK_VCI:PK_VCI + 512] = vc(c_im)
    def pl(a_gp):
        a = np.broadcast_to(a_gp[:, None, :], (64, 16, 64))
        return a.transpose(2, 0, 1).reshape(64, 1024)
    pk[0:64, PK_PLR:PK_PLR + 1024] = pl(lam_re)
    pk[0:64, PK_PLI:PK_PLI + 1024] = pl(lam_im)
    pk[0:64, PK_PLS:PK_PLS + 1024] = pl(ls_gp)
    pk[0:64, PK_PBR:PK_PBR + 1024] = b_re.transpose(1, 0, 2).reshape(64, 1024)
    pk[0:64, PK_PBI:PK_PBI + 1024] = b_im.transpose(1, 0, 2).reshape(64, 1024)
    pk[0:64, PK_PCR:PK_PCR + 1024] = c_re.transpose(2, 0, 1).reshape(64, 1024)
    pk[0:64, PK_PCI:PK_PCI + 1024] = c_im.transpose(2, 0, 1).reshape(64, 1024)
    pk[:, PK_D:PK_D + KT] = d_s5.reshape(KT, 128).T
    return pk


def host_nw(pre_mix, post_mix, pre_mlp, post_mlp):
    nw = np.zeros((128, 4 * 4 * KT), np.float32)
    for li in range(4):
        for j, a in enumerate((pre_mix, post_mix, pre_mlp, post_mlp)):
            nw[:, (li * 4 + j) * KT:(li * 4 + j + 1) * KT] = np.asarray(a[li], np.float32).reshape(KT, 128).T
    return nw


def host_inmap(inp, seq, L, layers, mixers=True):
    f = lambda a: np.ascontiguousarray(np.asarray(a, dtype=np.float32))
    odd = [li for li in layers if li % 2 == 1]
    even = [li for li in layers if li % 2 == 0]
    m = {
        "xT": np.ascontiguousarray(f(inp["x"])[seq, :L, :].T),
        "nw": host_nw(inp["pre_mix_norm"], inp["post_mix_norm"], inp["pre_mlp_norm"], inp["post_mlp_norm"]),
        "w_up": f(inp["w_up"])[layers],
        "w_down": f(inp["w_down"])[layers],
        "cn": host_consts(),
    }
    if mixers:
        js = [li // 2 for li in odd] or [0]
        m["s5pack"] = np.stack([host_s5pack(*(f(inp[k])[j] for k in
                                ("lam_re", "lam_im", "log_step", "b_re", "b_im", "c_re", "c_im", "d_s5"))) for j in js])
        m["w_glu_a"] = f(inp["w_glu_a"])[js]
        m["w_glu_b"] = f(inp["w_glu_b"])[js]
        host_even(m, inp, [li // 2 for li in even] or [0], L)
    return m


def host_even(m, inp, js, L):
    f = lambda a: np.ascontiguousarray(np.asarray(a, dtype=np.float32))
    m["epack"] = np.stack([host_epack(*(f(inp[k])[j] for k in ("conv_w", "conv_b", "dt_bias", "a_log", "d_ssd", "ssd_norm")))
                           for j in js])
    m["w_in"] = f(inp["w_in"])[js]
    m["w_out"] = f(inp["w_out"])[js]
    m["rot"] = host_rot(L)


_PROG_CACHE = {}


def kernel(**inputs):
    x = np.asarray(inputs["x"], dtype=np.float32)
    B, L, _ = x.shape
    layers = [0, 1, 2, 3]
    key = (L,)
    if key not in _PROG_CACHE:
        P = Prog(L, layers=layers, mixers=True)
        _PROG_CACHE[key] = P.build()
    nc = _PROG_CACHE[key]
    shared = host_inmap(inputs, 0, L, layers)
    in_maps = []
    for core in range(NCORES):
        seq = core % B
        m = dict(shared)
        m["xT"] = np.ascontiguousarray(x[seq].T)
        in_maps.append(m)
    res = run_bass_kernel_spmd(nc, in_maps, core_ids=list(range(NCORES)))
    out = np.stack([np.ascontiguousarray(np.asarray(res.results[b]["outT"]).T) for b in range(B)])
    return out.astype(np.float32)
```
